# Optimizing a Trainium2 kernel written in Bass

```python
import math
import jax, jax.numpy as jnp
from jax import lax
import numpy as np

D_MODEL = 1024
BATCH = 4
SEQ = 8192
DEPTH = 2

HEAD_DIM = 64
BRANCH_W = D_MODEL // 4
N_HEADS_BR = BRANCH_W // HEAD_DIM
N_ATTN_HEADS = 2 * N_HEADS_BR
MOBA_BLOCK = 256
MOBA_TOPK = 3
MOBA_QCHUNK = 128
S5_GROUP = 16
S5_GROUPS = BRANCH_W // S5_GROUP
S5_STATE = 64
HGRN_CHUNK = 64
DIL_PAIRS = ((128, 1), (512, 4), (2048, 16))
DIL_BLOCK = 128
REL_BUCKETS = 32
REL_MAX_DIST = 2048
ALPHA = (2 * DEPTH) ** 0.25
BETA = (8 * DEPTH) ** -0.25
N_IN_SLOTS = 10
IN_COLS = N_IN_SLOTS * BRANCH_W + D_MODEL
LN_EPS = 1e-5
RMS_EPS = 1e-6
NEG = -1e30

kernel_name = 'hymba_moba_s5_hgrn2_dilated_deepnorm'


def rel_bucket(dist):
    max_exact = REL_BUCKETS // 2
    d = jnp.maximum(dist, 1).astype(jnp.float32)
    large = max_exact + (jnp.log(d / max_exact) / math.log(REL_MAX_DIST / max_exact)
                         * (REL_BUCKETS - max_exact)).astype(jnp.int32)
    return jnp.where(dist < max_exact, dist, jnp.minimum(large, REL_BUCKETS - 1))


def layer_norm(h, g, b):
    h = h.astype(jnp.float32)
    mu = h.mean(-1, keepdims=True)
    var = jnp.square(h - mu).mean(-1, keepdims=True)
    return (h - mu) * lax.rsqrt(var + LN_EPS) * g.astype(jnp.float32) + b.astype(jnp.float32)


def moba_attention(q, k, v, bias_table):
    Bt, H, S, Dh = q.shape
    scale = Dh ** -0.5
    Sp = -(-S // MOBA_BLOCK) * MOBA_BLOCK
    nkb = Sp // MOBA_BLOCK
    pad = ((0, 0), (0, 0), (0, Sp - S), (0, 0))
    kblk = jnp.pad(k, pad).reshape(Bt, H, nkb, MOBA_BLOCK, Dh)
    vblk = jnp.pad(v, pad).reshape(Bt, H, nkb, MOBA_BLOCK, Dh)
    kmean = kblk.astype(jnp.float32).mean(axis=3)
    topk = max(1, min(MOBA_TOPK, nkb - 1))
    nqc = S // MOBA_QCHUNK
    q_chunks = q.reshape(Bt, H, nqc, MOBA_QCHUNK, Dh).transpose(2, 0, 1, 3, 4)
    b_idx = jnp.arange(Bt)[:, None, None, None]
    h_idx = jnp.arange(H)[None, :, None, None]
    in_blk = jnp.arange(MOBA_BLOCK)
    blk_ids = jnp.arange(nkb)

    def one_chunk(args):
        qi, c = args
        qf = qi.astype(jnp.float32)
        pos = c * MOBA_QCHUNK + jnp.arange(MOBA_QCHUNK)
        own = (c * MOBA_QCHUNK) // MOBA_BLOCK
        gate = jnp.einsum('bhqd,bhnd->bhqn', qf, kmean)
        gate = jnp.where(blk_ids < own, gate, -jnp.inf)
        _, idx = lax.top_k(gate, topk)
        valid = idx < own
        ksel = kblk[b_idx, h_idx, idx].astype(jnp.float32)
        vsel = vblk[b_idx, h_idx, idx].astype(jnp.float32)
        l_sel = jnp.einsum('bhqd,bhqtkd->bhqtk', qf, ksel) * scale
        sel_pos = idx[..., None] * MOBA_BLOCK + in_blk
        sel_dist = jnp.maximum(pos[:, None, None] - sel_pos, 0)
        l_sel = l_sel + bias_table[rel_bucket(sel_dist), h_idx[..., None]].astype(jnp.float32)
        l_sel = jnp.where(valid[..., None], l_sel, NEG)
        kown = lax.dynamic_index_in_dim(kblk, own, axis=2, keepdims=False).astype(jnp.float32)
        vown = lax.dynamic_index_in_dim(vblk, own, axis=2, keepdims=False).astype(jnp.float32)
        own_dist = pos[:, None] - (own * MOBA_BLOCK + in_blk)[None, :]
        own_bias = bias_table[rel_bucket(jnp.maximum(own_dist, 0))].astype(jnp.float32).transpose(2, 0, 1)
        l_own = jnp.einsum('bhqd,bhkd->bhqk', qf, kown) * scale + own_bias
        l_own = jnp.where(own_dist >= 0, l_own, NEG)
        n_sel = topk * MOBA_BLOCK
        logits = jnp.concatenate([l_sel.reshape(Bt, H, MOBA_QCHUNK, n_sel), l_own], axis=-1)
        p = jax.nn.softmax(logits, axis=-1)
        p_sel = p[..., :n_sel].reshape(Bt, H, MOBA_QCHUNK, topk, MOBA_BLOCK)
        return (jnp.einsum('bhqtk,bhqtkd->bhqd', p_sel, vsel)
                + jnp.einsum('bhqk,bhkd->bhqd', p[..., n_sel:], vown))

    out = lax.map(one_chunk, (q_chunks, jnp.arange(nqc, dtype=jnp.int32)))
    return out.transpose(1, 2, 0, 3, 4).reshape(Bt, H, S, Dh)


def dilated_attention(q, k, v, bias_table):
    Bt, H, S, Dh = q.shape
    scale = Dh ** -0.5
    i = jnp.arange(DIL_BLOCK)[:, None]
    j = jnp.arange(2 * DIL_BLOCK)[None, :]
    dist_sub = DIL_BLOCK + i - j
    band = (dist_sub >= 0) & (dist_sub <= DIL_BLOCK)
    outs, lses = [], []
    for window, dil in DIL_PAIRS:
        L = S // dil
        nb = -(-L // DIL_BLOCK)
        Lp = nb * DIL_BLOCK

        def to_sub(t):
            t = t.reshape(Bt, H, L, dil, Dh).transpose(0, 1, 3, 2, 4)
            return jnp.pad(t, ((0, 0), (0, 0), (0, 0), (0, Lp - L), (0, 0)))

        def band_keys(t):
            t = jnp.pad(t, ((0, 0), (0, 0), (0, 0), (DIL_BLOCK, 0), (0, 0)))
            t = t.reshape(Bt, H, dil, nb + 1, DIL_BLOCK, Dh)
            return jnp.concatenate([t[:, :, :, :-1], t[:, :, :, 1:]], axis=4)

        qb = to_sub(q).reshape(Bt, H, dil, nb, DIL_BLOCK, Dh).astype(jnp.float32)
        kb = band_keys(to_sub(k)).astype(jnp.float32)
        vb = band_keys(to_sub(v)).astype(jnp.float32)
        logits = jnp.einsum('bhrnqd,bhrnkd->bhrnqk', qb, kb) * scale
        bias = bias_table[rel_bucket(jnp.maximum(dist_sub, 0) * dil)].astype(jnp.float32)
        logits = logits + bias.transpose(2, 0, 1)[None, :, None, None]
        key_pos = jnp.arange(nb)[:, None, None] * DIL_BLOCK + j[None] - DIL_BLOCK
        mask = band[None] & (key_pos >= 0)
        logits = jnp.where(mask, logits, NEG)
        m = logits.max(-1, keepdims=True)
        p = jnp.exp(logits - m)
        s = p.sum(-1, keepdims=True)
        o = jnp.einsum('bhrnqk,bhrnkd->bhrnqd', p, vb) / s
        lse = (m + jnp.log(s))[..., 0]
        o = o.reshape(Bt, H, dil, Lp, Dh)[:, :, :, :L].transpose(0, 1, 3, 2, 4).reshape(Bt, H, S, Dh)
        lse = lse.reshape(Bt, H, dil, Lp)[:, :, :, :L].transpose(0, 1, 3, 2).reshape(Bt, H, S)
        outs.append(o)
        lses.append(lse)
    w = jax.nn.softmax(jnp.stack(lses, 0), axis=0)
    return jnp.einsum('pbhs,pbhsd->bhsd', w, jnp.stack(outs, 0))


def s5_mixer(u, a_re, a_im, log_dt, b_re, b_im, c_re, c_im, d_skip, glu_w, glu_b):
    Bt, S, _ = u.shape
    uf = u.astype(jnp.float32)
    ug = uf.reshape(Bt, S, S5_GROUPS, S5_GROUP)
    ar = a_re.astype(jnp.float32)
    ai = a_im.astype(jnp.float32)
    dt = jnp.exp(log_dt.astype(jnp.float32))[:, None]
    mag = jnp.exp(dt * ar)
    abar_re = mag * jnp.cos(dt * ai)
    abar_im = mag * jnp.sin(dt * ai)
    nr, ni = abar_re - 1.0, abar_im
    den = ar * ar + ai * ai
    zr = (nr * ar + ni * ai) / den
    zi = (ni * ar - nr * ai) / den
    br = b_re.astype(jnp.float32)
    bi = b_im.astype(jnp.float32)
    bbar_re = zr[..., None] * br - zi[..., None] * bi
    bbar_im = zr[..., None] * bi + zi[..., None] * br
    xr = jnp.einsum('bsgc,gpc->bsgp', ug, bbar_re)
    xi = jnp.einsum('bsgc,gpc->bsgp', ug, bbar_im)
    at_re = jnp.broadcast_to(abar_re, xr.shape)
    at_im = jnp.broadcast_to(abar_im, xr.shape)

    def combine(e1, e2):
        a1r, a1i, b1r, b1i = e1
        a2r, a2i, b2r, b2i = e2
        return (a2r * a1r - a2i * a1i, a2r * a1i + a2i * a1r,
                a2r * b1r - a2i * b1i + b2r, a2r * b1i + a2i * b1r + b2i)

    _, _, hr, hi = lax.associative_scan(combine, (at_re, at_im, xr, xi), axis=1)
    y = (jnp.einsum('bsgp,gcp->bsgc', hr, c_re.astype(jnp.float32))
         - jnp.einsum('bsgp,gcp->bsgc', hi, c_im.astype(jnp.float32)))
    y = y.reshape(Bt, S, BRANCH_W) + d_skip.astype(jnp.float32) * uf
    y = jax.nn.gelu(y)
    return y * jax.nn.sigmoid(y @ glu_w.astype(jnp.float32) + glu_b.astype(jnp.float32))


def hgrn2_mixer(q, f_pre, i_in, lower_bound):
    Bt, S, _ = q.shape
    C = HGRN_CHUNK
    nc = S // C
    lb = lower_bound.astype(jnp.float32)
    f = lb + (1.0 - lb) * jax.nn.sigmoid(f_pre.astype(jnp.float32))
    log_f = jnp.log(f)
    k = 1.0 - f
    qf = jax.nn.silu(q.astype(jnp.float32)) * HEAD_DIM ** -0.5

    def chunks(t):
        return t.reshape(Bt, nc, C, N_HEADS_BR, HEAD_DIM).transpose(1, 0, 3, 2, 4)

    causal = jnp.tril(jnp.ones((C, C), dtype=bool))

    def step(state, inp):
        qc, kc, vc, gc = inp
        b = jnp.cumsum(gc, axis=2)
        diff = b[:, :, :, None, :] - b[:, :, None, :, :]
        decay = jnp.exp(jnp.where(causal[:, :, None], diff, NEG))
        scores = jnp.einsum('bhtd,bhsd,bhtsd->bhts', qc, kc, decay)
        o = (jnp.einsum('bhts,bhsv->bhtv', scores, vc)
             + jnp.einsum('bhtd,bhdv->bhtv', qc * jnp.exp(b), state))
        b_last = b[:, :, -1:]
        new_state = (jnp.exp(b_last[:, :, 0])[..., None] * state
                     + jnp.einsum('bhsd,bhsv->bhdv', kc * jnp.exp(b_last - b), vc))
        return new_state, o

    state0 = jnp.zeros((Bt, N_HEADS_BR, HEAD_DIM, HEAD_DIM), jnp.float32)
    _, o = lax.scan(step, state0, (chunks(qf), chunks(k), chunks(i_in.astype(jnp.float32)), chunks(log_f)))
    return o.transpose(1, 0, 3, 2, 4).reshape(Bt, S, BRANCH_W)


def setup_inputs(seed: int = 0) -> dict:
    key = jax.random.key(seed)
    ks = jax.random.split(key, 20)
    D, G, P, C = D_MODEL, S5_GROUPS, S5_STATE, S5_GROUP

    def nrm(k, shape, s):
        return jax.random.normal(k, shape, jnp.float32) * s

    n = jnp.arange(P, dtype=jnp.float32)
    return {
        'x': nrm(ks[0], (BATCH, SEQ, D), 1.0),
        'w_in': nrm(ks[1], (DEPTH, D, IN_COLS), D ** -0.5),
        'rel_bias': nrm(ks[2], (REL_BUCKETS, N_ATTN_HEADS), 0.2),
        's5_a_re': -0.5 + nrm(ks[3], (DEPTH, G, P), 0.01),
        's5_a_im': jnp.pi * n + nrm(ks[4], (DEPTH, G, P), 0.01),
        's5_log_dt': jax.random.uniform(ks[5], (DEPTH, G), jnp.float32, math.log(1e-3), math.log(1e-1)),
        's5_b_re': nrm(ks[6], (DEPTH, G, P, C), (2 * C) ** -0.5),
        's5_b_im': nrm(ks[7], (DEPTH, G, P, C), (2 * C) ** -0.5),
        's5_c_re': nrm(ks[8], (DEPTH, G, C, P), P ** -0.5),
        's5_c_im': nrm(ks[9], (DEPTH, G, C, P), P ** -0.5),
        's5_d': nrm(ks[10], (DEPTH, BRANCH_W), 0.5),
        's5_glu_w': nrm(ks[11], (DEPTH, BRANCH_W, BRANCH_W), BRANCH_W ** -0.5),
        's5_glu_b': nrm(ks[12], (DEPTH, BRANCH_W), 0.01),
        'hgrn_lower': nrm(ks[13], (DEPTH, BRANCH_W), 0.1),
        'branch_gain': 1.0 + nrm(ks[14], (DEPTH, D), 0.02),
        'w_out': nrm(ks[15], (DEPTH, D, D), D ** -0.5 * BETA),
        'ln_g': 1.0 + nrm(ks[16], (DEPTH, D), 0.02),
        'ln_b': nrm(ks[17], (DEPTH, D), 0.02),
    }


def reference(x, w_in, rel_bias, s5_a_re, s5_a_im, s5_log_dt, s5_b_re, s5_b_im, s5_c_re, s5_c_im,
              s5_d, s5_glu_w, s5_glu_b, hgrn_lower, branch_gain, w_out, ln_g, ln_b):
    Bt, S, D = x.shape
    W = BRANCH_W
    p_lb = jax.nn.softmax(hgrn_lower.astype(jnp.float32), axis=0)
    lb_all = jnp.cumsum(p_lb, axis=0) - p_lb[0]

    def heads(t):
        return t.reshape(Bt, S, N_HEADS_BR, HEAD_DIM).transpose(0, 2, 1, 3)

    def merge(t):
        return t.transpose(0, 2, 1, 3).reshape(Bt, S, W)

    for l in range(DEPTH):
        proj = x @ w_in[l]
        qa, ka, va, us, qc, fc, ic, qd, kd, vd = [proj[..., s * W:(s + 1) * W] for s in range(N_IN_SLOTS)]
        gates = proj[..., N_IN_SLOTS * W:]
        ya = merge(moba_attention(heads(qa), heads(ka), heads(va), rel_bias[:, :N_HEADS_BR]))
        yb = s5_mixer(us, s5_a_re[l], s5_a_im[l], s5_log_dt[l], s5_b_re[l], s5_b_im[l],
                      s5_c_re[l], s5_c_im[l], s5_d[l], s5_glu_w[l], s5_glu_b[l])
        yc = hgrn2_mixer(qc, fc, ic, lb_all[l])
        yd = merge(dilated_attention(heads(qd), heads(kd), heads(vd), rel_bias[:, N_HEADS_BR:]))
        y = jnp.concatenate([ya, yb, yc, yd], axis=-1).astype(jnp.float32)
        yg = y.reshape(Bt, S, D // HEAD_DIM, HEAD_DIM)
        yg = yg * lax.rsqrt(jnp.mean(yg * yg, axis=-1, keepdims=True) + RMS_EPS)
        y = (yg.reshape(Bt, S, D) * branch_gain[l].astype(jnp.float32)
             * jax.nn.silu(gates.astype(jnp.float32)))
        y = y.astype(x.dtype) @ w_out[l]
        x = layer_norm(ALPHA * x + y, ln_g[l], ln_b[l]).astype(x.dtype)
    return x
```

```python
import math
import numpy as np
import ml_dtypes
import concourse.bass as bass
import concourse.mybir as mybir
from concourse.bass_utils import run_bass_kernel_spmd

F32 = mybir.dt.float32
BF16 = mybir.dt.bfloat16
AF = mybir.ActivationFunctionType
ALU = mybir.AluOpType
AX = mybir.AxisListType

D_MODEL = 1024
DEPTH = 2
ALPHA = (2 * DEPTH) ** 0.25
NEGM = -30000.0
WA = 2432
WD = 2944
SKOFF = 384
T5 = 256


class Buf:
    __slots__ = ("w", "r", "x")

    def __init__(self, x=False):
        self.w = None
        self.r = {}
        self.x = x


class KB:
    ENG = ("pe", "dve", "act", "pool", "sp")
    ND = 8

    def __init__(self, nc):
        self.nc = nc
        self.eng = {"pe": nc.tensor, "dve": nc.vector, "act": nc.scalar, "pool": nc.gpsimd, "sp": nc.sync}
        self.sem = {e: nc.alloc_semaphore(name="sem_" + e) for e in self.ENG}
        for q in range(self.ND):
            self.sem[("d", q)] = nc.alloc_semaphore(name="dsem%d" % q)
        self.cnt = {k: 0 for k in self.sem}
        self.known = {k: {} for k in self.sem}
        self.snap = {k: {} for k in self.sem}
        self.nins = 0

    def _need(self, e, reads, writes):
        need = {}
        for b in reads:
            if b.w is not None:
                x, v = b.w
                need[x] = max(need.get(x, 0), v)
        for b in writes:
            if b.w is not None:
                x, v = b.w
                need[x] = max(need.get(x, 0), v)
            for x, v in b.r.items():
                need[x] = max(need.get(x, 0), v)
        kn = self.known[e]
        for x, v in need.items():
            if x == "pe" and e == "pe":
                continue
            if isinstance(x, tuple):
                v = self.cnt[x]
            if kn.get(x, 0) >= v:
                continue
            self.eng[e].wait_ge(self.sem[x], v)
            self.nins += 1
            kn[x] = v
            sn = self.snap[x].get(v)
            if sn:
                for y, u in sn.items():
                    if kn.get(y, 0) < u:
                        kn[y] = u

    def op(self, e, fn, reads, writes):
        xr = [b for b in reads if b.x]
        if xr:
            writes = list(writes) + xr
        self._need(e, reads, writes)
        ins = fn(self.eng[e])
        self.cnt[e] += 1
        c = self.cnt[e]
        ins.then_inc(self.sem[e], 1)
        self.nins += 1
        self.snap[e][c] = dict(self.known[e])
        for b in reads:
            b.r[e] = c
        for b in writes:
            b.w = (e, c)
            b.r = {}

    def dma(self, e, out, in_, reads, writes, q):
        self._need(e, reads, writes)
        ins = self.eng[e].dma_start(out=out, in_=in_)
        key = ("d", q)
        self.cnt[key] += 16
        c = self.cnt[key]
        ins.then_inc(self.sem[key], 16)
        self.nins += 1
        self.snap[key][c] = dict(self.known[e])
        for b in reads:
            b.r[key] = c
        for b in writes:
            b.w = (key, c)
            b.r = {}

    def barrier(self):
        for e in self.ENG:
            for x in self.sem:
                if x == e and e == "pe":
                    continue
                v = self.cnt[x]
                if v > self.known[e].get(x, 0):
                    self.eng[e].wait_ge(self.sem[x], v)
                    self.nins += 1
                    self.known[e][x] = v

    def wait_all_dma(self, e):
        for q in range(self.ND):
            x = ("d", q)
            if self.cnt[x] > self.known[e].get(x, 0):
                self.eng[e].wait_ge(self.sem[x], self.cnt[x])
                self.known[e][x] = self.cnt[x]

    def finish(self, e, bufs):
        self._need(e, bufs, [])

    def mm(self, out, lhsT, rhs, R, W, start=True, stop=True):
        self.op("pe", lambda g: g.matmul(out, lhsT=lhsT, rhs=rhs, start=start, stop=stop), R, W)

    def tr(self, out, in_, ident, R, W):
        self.op("pe", lambda g: g.transpose(out, in_, ident), R, W)

    def act(self, out, in_, func, R, W, scale=1.0, bias=0.0, accum=None):
        if accum is None:
            self.op("act", lambda g: g.activation(out, in_, func, bias=bias, scale=scale), R, W)
        else:
            self.op("act", lambda g: g.activation(out, in_, func, bias=bias, scale=scale, accum_out=accum), R, W)

    def tt(self, e, out, a, b, op, R, W):
        self.op(e, lambda g: g.tensor_tensor(out, a, b, op), R, W)

    def ts(self, e, out, a, s1, op0, R, W, s2=None, op1=None):
        if op1 is None:
            self.op(e, lambda g: g.tensor_scalar(out, a, s1, None, op0), R, W)
        else:
            self.op(e, lambda g: g.tensor_scalar(out, a, s1, s2, op0, op1), R, W)

    def stt(self, out, in0, scalar, in1, op0, op1, R, W):
        self.op("dve", lambda g: g.scalar_tensor_tensor(out, in0, scalar, in1, op0, op1), R, W)

    def cp(self, e, out, in_, R, W):
        self.op(e, lambda g: g.tensor_copy(out, in_), R, W)

    def memset(self, e, ap, v, W):
        self.op(e, lambda g: g.memset(ap, v), [], W)


def _rel_bucket_np(dist):
    d = np.maximum(dist, 1).astype(np.float32)
    large = 16 + (np.log(d / np.float32(16)) / np.float32(math.log(2048 / 16)) * np.float32(16)).astype(np.int32)
    return np.where(dist < 16, dist, np.minimum(large, 31)).astype(np.int64)


def _skew_tables():
    k = np.arange(128)[:, None]
    dA = np.arange(WA)[None, :] - k - SKOFF
    idxA = np.where(dA >= 0, _rel_bucket_np(np.maximum(dA, 0)), 32)
    cA = np.where(dA >= 0, 0.0, NEGM).astype(np.float32)
    dD = np.arange(WD)[None, :] - k - SKOFF
    mult = ((dD <= 128).astype(np.int64) + ((dD % 4 == 0) & (dD <= 512)) + ((dD % 16 == 0) & (dD <= 2048)))
    okD = (dD >= 0) & (dD <= 2048) & (mult >= 1)
    idxD = np.where(okD, _rel_bucket_np(np.maximum(dD, 0)), 32)
    cD = np.where(okD, np.log(np.maximum(mult, 1).astype(np.float64)), NEGM).astype(np.float32)
    return idxA, cA, idxD, cD


_CONST_CACHE = {}


def _consts():
    if "c" in _CONST_CACHE:
        return _CONST_CACHE["c"]
    c = {}
    c["id"] = np.eye(128, dtype=np.float32)
    sw = np.zeros((128, 128), np.float32)
    for p in range(64):
        sw[p, 64 + p] = 1.0
        sw[64 + p, p] = 1.0
    c["sw"] = sw
    c["sgn"] = np.where(np.arange(128) < 64, 1.0, -1.0).astype(np.float32)[:, None]
    seg = np.ones((128, T5), np.float32)
    seg[:, ::64] = 0.0
    c["seg"] = seg
    s = (np.arange(128) % 64)[:, None]
    t = np.arange(64)[None, :]
    c["m01"] = np.concatenate([(t >= s), (t >= s)], axis=1).astype(np.float32)
    c["iota"] = np.broadcast_to(np.arange(T5 + 1, dtype=np.float32)[None, :], (128, T5 + 1)).copy()
    wb65 = np.zeros((128, 64), np.float32)
    wb65[:64, :] = 1.0 / 64
    wb65[64, :] = 1e-6
    c["wb65"] = wb65
    wb2 = np.zeros((128, 128), np.float32)
    wb2[:64, :64] = 1.0 / 64
    wb2[64:, 64:] = 1.0 / 64
    c["wb2"] = wb2
    c["eps"] = np.full((128, 1), 1e-6, np.float32)
    c["ones"] = np.ones((128, 128), np.float32)
    names = ["id", "sw", "sgn", "seg", "m01", "iota", "wb65", "wb2", "eps", "ones"]
    off = {}
    o = 0
    for n in names:
        off[n] = (o, c[n].shape[1])
        o += c[n].shape[1]
    arr = np.concatenate([c[n] for n in names], axis=1).astype(np.float32)
    _CONST_CACHE["c"] = (arr, off)
    return arr, off


def _pad_idx():
    idx = -np.ones((4, 128), np.int64)
    for hi in range(4):
        for a in range(4):
            for sl in range(16):
                idx[hi, 32 * a + sl] = 16 * (a + 4 * hi) + sl
    return idx


def host_stageA_inputs(l, b, hf, S, x_T, w_in, rel_bias, s5_a_re, s5_a_im, s5_log_dt, s5_b_re, s5_b_im,
                       s5_c_re, s5_c_im, s5_d, s5_glu_w, s5_glu_b, hgrn_lower, branch_gain):
    W = 256
    cs = slice(128 * hf, 128 * hf + 128)
    pidx = _pad_idx()
    win = w_in[l]
    zc = np.zeros((D_MODEL, 1), np.float32)

    def slot(sidx):
        return win[:, sidx * W:(sidx + 1) * W][:, cs]

    gates = win[:, 10 * W:]
    gA, gB, gC, gD = [gates[:, i * W:(i + 1) * W][:, cs] for i in range(4)]

    def padded(m, his):
        mz = np.concatenate([m, zc], axis=1)
        return np.concatenate([mz[:, pidx[h_]] for h_ in his], axis=1)

    qa, ka, va, us, qc, fc, ic, qd, kd, vd = [slot(i) for i in range(10)]
    cols = [qa[:, :64], qa[:, 64:], ka[:, :64], ka[:, 64:], gA[:, :64], gA[:, 64:], gD[:, :64], gD[:, 64:],
            qd, kd, qc, fc, gC, padded(win[:, 3 * W:4 * W], [2 * hf, 2 * hf + 1, 2 - 2 * hf, 3 - 2 * hf]), padded(gates[:, W:2 * W], [2 * hf, 2 * hf + 1]), va, vd, ic]
    w = np.ascontiguousarray(np.concatenate(cols, axis=1), dtype=np.float32)
    assert w.shape == (1024, 2304)
    idxA, cA, idxD, cD = _skew_tables()
    rbz = np.concatenate([rel_bias, np.zeros((1, 8), np.float32)], axis=0)
    hA = [2 * hf, 2 * hf + 1]
    hD = [4 + 2 * hf, 4 + 2 * hf + 1]
    skA = np.stack([rbz[idxA, h] for h in hA], 0).astype(np.float32)
    skD = np.stack([rbz[idxD, h] for h in hD], 0).astype(np.float32)
    b31 = np.broadcast_to(rel_bias[31, hA][None, :], (128, 2)).astype(np.float32).copy()
    perm = [2 * hf, 2 * hf + 1, 2 - 2 * hf, 3 - 2 * hf]
    gmap = [(g % 4) + 4 * perm[g // 4] for g in range(16)]

    def ap_layout(m):
        o = np.zeros((128, 4, 64), np.float32)
        for hi in range(4):
            for a in range(4):
                o[32 * a:32 * a + 32, hi, :] = m[a + 4 * perm[hi]][None, :]
        return o

    def bT_layout(bm):
        o = np.zeros((128, 4, 64), np.float32)
        for hi in range(4):
            for a in range(4):
                o[32 * a:32 * a + 16, hi, :] = bm[a + 4 * perm[hi]].T
        return o

    are = s5_a_re[l]
    aim = s5_a_im[l]
    ldt = s5_log_dt[l]
    ldtb = np.broadcast_to(ldt[:, None], (16, 64))
    s5p1 = np.stack([bT_layout(s5_b_re[l]), bT_layout(s5_b_im[l]), ap_layout(are), ap_layout(aim), ap_layout(ldtb)], axis=1)
    st2 = lambda m: np.concatenate([m[gmap].T, m[gmap].T], axis=0)
    s5p2 = np.stack([st2(are), st2(aim), st2(ldtb)], axis=1)
    cre = s5_c_re[l]
    cim = s5_c_im[l]
    CA = np.zeros((128, 16, 64), np.float32)
    CB = np.zeros((128, 16, 64), np.float32)
    for g in range(16):
        o_ = 32 if g % 4 == 3 else 0
        CA[:64, g, o_:o_ + 16] = cre[gmap[g]].T
        CA[64:, g, o_:o_ + 16] = cim[gmap[g]].T
        CB[:64, g, o_:o_ + 16] = cim[gmap[g]].T
        CB[64:, g, o_:o_ + 16] = cre[gmap[g]].T
    s5c = np.stack([CA, CB], axis=1)

    def padvec(v, his):
        vz = np.concatenate([v, np.zeros(1, np.float32)])
        return np.stack([vz[pidx[h_]] for h_ in his], axis=1)

    own = [2 * hf, 2 * hf + 1]
    dsk = padvec(s5_d[l], perm)
    gb = padvec(s5_glu_b[l], own)
    gain = np.zeros((128, 8), np.float32)
    bg = branch_gain[l]
    gain[:64, 0] = bg[0 * W + 128 * hf: 0 * W + 128 * hf + 64]
    gain[:64, 1] = bg[0 * W + 128 * hf + 64: 0 * W + 128 * hf + 128]
    gain[:, 2:4] = padvec(bg[1 * W:2 * W], own)
    gain[:, 4] = bg[2 * W:3 * W][cs]
    gain[:64, 5] = bg[3 * W + 128 * hf: 3 * W + 128 * hf + 64]
    gain[:64, 6] = bg[3 * W + 128 * hf + 64: 3 * W + 128 * hf + 128]
    hl = np.ascontiguousarray(hgrn_lower[:, cs].T).astype(np.float32)
    small = np.zeros((128, 24), np.float32)
    small[:, 0:2] = b31; small[:, 2:6] = dsk; small[:, 6:8] = gb; small[:, 8:16] = gain; small[:, 16:18] = hl; small[:, 18] = float(l)
    gwz = np.zeros((257, 257), np.float32); gwz[:256, :256] = s5_glu_w[l]
    gluw = np.stack([gwz[pidx[perm[hi]]][:, np.concatenate([pidx[own[0]], pidx[own[1]]])] for hi in range(4)], axis=1)
    E = np.zeros((32, S), np.float32)
    for n in range(min(32, S // 256)):
        E[n, 256 * n:256 * n + 256] = 1.0
    carr, _ = _consts()
    return {"xT": np.ascontiguousarray(x_T), "w": w, "skA": skA, "skD": skD,
            "skAc": cA, "skDc": cD, "s5p1": np.ascontiguousarray(s5p1), "s5p2": np.ascontiguousarray(s5p2),
            "s5c": np.ascontiguousarray(s5c), "small": small, "gluw": np.ascontiguousarray(gluw, dtype=np.float32), "E": E.astype(ml_dtypes.bfloat16), "cst": carr}


NCOL = 2304
C_QA, C_KA, C_GA, C_GD = 0, 128, 256, 384
C_QD, C_KD, C_QC, C_FC, C_GC = 512, 640, 768, 896, 1024
C_U, C_GB, C_TM = 1152, 1664, 1920


def build_stageA(S, layer, stop_after=None, branches="ABCD"):
    nc = bass.Bass("TRN2", target_bir_lowering=False)
    kb = KB(nc)
    NST = S // 512
    NKT = S // 128
    carr, coff = _consts()
    NCST = carr.shape[1]
    dr = lambda n, sh, dt=F32, kind="ExternalInput": nc.dram_tensor(n, sh, dt, kind=kind).ap()
    xT = dr("xT", [1024, S])
    wd = dr("w", [1024, NCOL])
    skA_d = dr("skA", [2, 128, WA]); skD_d = dr("skD", [2, 128, WD])
    skAc_d = dr("skAc", [128, WA]); skDc_d = dr("skDc", [128, WD])
    s5p1_d = dr("s5p1", [128, 5, 4, 64]); s5p2_d = dr("s5p2", [128, 3, 16]); s5c_d = dr("s5c", [128, 2, 16, 64])
    small_d = dr("small", [128, 24]); E_d = dr("E", [32, S], BF16); cst_d = dr("cst", [128, NCST])
    gw_d = dr("gluw", [128, 4, 256])
    yg = dr("yg", [512, S], BF16, kind="ExternalOutput")

    sb = lambda n, sh, dt=F32: nc.alloc_sbuf_tensor("s_" + n, sh, dt)
    cst = sb("cst", [128, NCST]); B_cst = Buf()
    cs = lambda n: cst[:, coff[n][0]:coff[n][0] + coff[n][1]]
    small = sb("small", [128, 24]); B_small = Buf()
    wbf = sb("wbf", [128, 8, NCOL], BF16); B_w = Buf()
    KA = [sb("KA%d" % h, [96, S], BF16) for h in range(2)]
    B_KA = [[Buf() for _ in range(NST)] for h in range(2)]; B_E = Buf()
    VA = sb("VA", [128, NKT, 2, 65], BF16); B_VA = [Buf() for _ in range(NST)]
    KD = sb("KD", [128, 5, 512], BF16); B_KD = [Buf() for _ in range(5)]
    VD = sb("VD", [128, 20, 2, 65], BF16); B_VD = [Buf() for _ in range(5)]
    skA = sb("skA", [128, 2, WA], BF16); skD = sb("skD", [128, 2, WD], BF16); B_sk = Buf()
    ksum = [sb("ksum%d" % h, [64, 32]) for h in range(2)]; B_ksum = [Buf(), Buf()]
    gsb = [sb("gsb%d" % h, [128, 32]) for h in range(2)]; B_gsb = [Buf(), Buf()]
    tab = sb("tab", [128, 16, 2, T5 + 1], BF16); B_tab = Buf()
    Bm = sb("Bm", [128, 4, 2, 128], BF16); B_Bm = Buf()
    Cm = sb("Cm", [128, 16, 2, 64], BF16); B_Cm = Buf(); BmX = sb("BmX", [128, 4, 2, 128], BF16)
    cs2 = sb("cs2", [128, 16, 2]); B_Rot = Buf()
    rr = sb("rr", [128, 16]); B_rr = Buf()
    carry = sb("carry", [128, 16]); B_carry = [Buf() for _ in range(16)]
    gw = sb("gw", [128, 4, 256], BF16); B_gw = Buf()
    hst = sb("hst", [128, 64]); hstb = sb("hstb", [128, 2, 64], BF16); B_hst = Buf(); B_hstb = Buf()
    lbv = sb("lbv", [128, 2]); B_lb = Buf()
    idb = sb("idb", [128, 128], BF16); B_idb = Buf()
    ps = [nc.alloc_psum_tensor("ps%d" % i, [128, 512], F32) for i in range(7)]
    psb = nc.alloc_psum_tensor("psb", [128, 1024], BF16)
    B_ps = [Buf(True) for _ in range(7)]; B_psb = Buf(True)
    B_ps6 = [B_ps[6], B_ps[6], B_ps[6]]
    pp = [0]

    def pingpong():
        pp[0] ^= 1
        return ps[pp[0]], B_ps[pp[0]]

    kb.dma("sp", cst[:], cst_d[:, :], [], [B_cst], 2)
    kb.dma("sp", small[:], small_d[:, :], [], [B_small], 2)
    for h in range(2):
        kb.dma("sp", KA[h][64:96, :], E_d[:, :], [], [B_E], 2)
    b31 = small[:, 0:2]; dsk = small[:, 2:6]; glub = small[:, 6:8]; gain = small[:, 8:16]; hl = small[:, 16:18]
    eps = cs("eps")

    ang2 = sb("ang2", [128, T5 + 1])
    B_stg = Buf(); B_stg2 = Buf(); B_p = Buf(); B_tm = Buf(); B_ang = Buf()
    with nc.sbuf_tensor("t_stg", [128, WD], F32) as stg, nc.sbuf_tensor("t_stg2", [128, WD], F32) as stg2:
        wv = wd.rearrange("(k p) n -> p k n", p=128)
        for dk in range(8):
            kb.dma("sp", stg[:, 0:NCOL], wv[:, dk, :], [], [B_stg], 1)
            kb.cp("pool" if dk % 2 else "dve", wbf[:, dk, :], stg[:, 0:NCOL], [B_stg], [B_w])
        for i in range(4):
            kb.dma("sp", stg[:, 0:256], gw_d[:, i, :], [], [B_stg], 1)
            kb.cp("dve", gw[:, i, :], stg[:, 0:256], [B_stg], [B_gw])
        kb.cp("dve", idb[:], cs("id"), [B_cst], [B_idb])
        kb.dma("sp", stg2[:, 0:WA], skAc_d[:, :], [], [B_stg2], 1)
        for h in range(2):
            kb.dma("sp", stg[:, 0:WA], skA_d[h, :, :], [], [B_stg], 1)
            kb.tt("dve", skA[:, h, :], stg[:, 0:WA], stg2[:, 0:WA], ALU.add, [B_stg, B_stg2], [B_sk])
        kb.dma("sp", stg2[:, 0:WD], skDc_d[:, :], [B_sk], [B_stg2], 1)
        for h in range(2):
            kb.dma("sp", stg[:, 0:WD], skD_d[h, :, :], [], [B_stg], 1)
            kb.tt("dve", skD[:, h, :], stg[:, 0:WD], stg2[:, 0:WD], ALU.add, [B_stg, B_stg2], [B_sk])
        kb.tt("dve", lbv[:, 1:2], hl[:, 1:2], hl[:, 0:1], ALU.subtract, [B_small], [B_lb])
        kb.act(lbv[:, 0:1], lbv[:, 1:2], AF.Sigmoid, [B_lb], [B_lb])
        kb.tt("dve", lbv[:, 0:1], lbv[:, 0:1], small[:, 18:19], ALU.mult, [B_lb, B_small], [B_lb])
        kb.ts("dve", lbv[:, 1:2], lbv[:, 0:1], -1.0, ALU.mult, [B_lb], [B_lb], s2=1.0, op1=ALU.add)
    kb.barrier()
    with nc.sbuf_tensor("t_p1", [128, 5, 4, 64], F32) as p1, nc.sbuf_tensor("t_p2", [128, 3, 16], F32) as p2, \
            nc.sbuf_tensor("t_c5", [128, 2, 16, 64], F32) as c5, nc.sbuf_tensor("t_tm", [128, 20, 256], F32) as tm, \
            nc.sbuf_tensor("t_ang", [128, T5 + 1], F32) as ang, nc.sbuf_tensor("t_angi", [128, T5 + 1], mybir.dt.int32) as angi:
        kb.dma("sp", p1[:], s5p1_d[:, :, :, :], [], [B_p], 2)
        kb.dma("sp", p2[:], s5p2_d[:, :, :], [], [B_p], 2)
        kb.dma("sp", c5[:], s5c_d[:, :, :, :], [], [B_p], 2)
        T = lambda i: tm[:, i, :]
        V4 = lambda a: a.rearrange("p (h q) -> p h q", h=4)
        BRT, BIT, AR, AI, LDT = [p1[:, i, :, :] for i in range(5)]
        R_, W_ = [B_p, B_tm], [B_tm]
        TWO_PI = 2.0 * math.pi

        def sincos(dst_sin, dst_cos, a_in, n):
            for dst, shift in ((dst_sin, 0.0), (dst_cos, 0.25)):
                A = ang[:, 0:n]; Ai = angi[:, 0:n]
                if len(a_in.shape) == 3:
                    A = V4(A); Ai = V4(Ai)
                kb.ts("dve", A, a_in, 1.0 / TWO_PI, ALU.mult, R_ + [B_ang, B_rr], [B_ang], s2=shift, op1=ALU.add)
                kb.cp("dve", Ai, A, [B_ang], [B_ang])
                kb.op("dve", lambda g: g.tensor_copy(ang2v(n, a_in), Ai), [B_ang], [B_ang])
                kb.tt("dve", A, A, ang2v(n, a_in), ALU.subtract, [B_ang], [B_ang])
                kb.ts("dve", ang2v(n, a_in), A, 0.5, ALU.is_gt, [B_ang], [B_ang])
                kb.tt("dve", A, A, ang2v(n, a_in), ALU.subtract, [B_ang], [B_ang])
                kb.ts("dve", ang2v(n, a_in), A, -0.5, ALU.is_lt, [B_ang], [B_ang])
                kb.tt("dve", A, A, ang2v(n, a_in), ALU.add, [B_ang], [B_ang])
                kb.act(dst, A, AF.Sin, [B_ang], W_ + [B_tab], scale=TWO_PI)

        def ang2v(n, a_in):
            v = ang2[:, 0:n]
            return V4(v) if len(a_in.shape) == 3 else v

        dt_ = V4(T(0)); dar = V4(T(1)); dai = V4(T(2)); mag = V4(T(3)); sn = V4(T(4)); csn = V4(T(5))
        lre = V4(T(6)); lim = V4(T(7)); den = V4(T(8)); t9 = V4(T(9)); zre = V4(T(10)); zim = V4(T(11))
        bre = V4(T(12)); bim = V4(T(13)); t14 = V4(T(14))
        kb.act(dt_, LDT, AF.Exp, R_, W_)
        kb.tt("dve", dar, dt_, AR, ALU.mult, R_, W_)
        kb.tt("dve", dai, dt_, AI, ALU.mult, R_, W_)
        kb.act(mag, dar, AF.Exp, R_, W_)
        sincos(sn, csn, dai, 256)
        kb.tt("dve", lre, mag, csn, ALU.mult, R_, W_)
        kb.tt("dve", lim, mag, sn, ALU.mult, R_, W_)
        kb.ts("dve", lre, lre, -1.0, ALU.add, R_, W_)
        kb.tt("dve", den, AR, AR, ALU.mult, R_, W_)
        kb.tt("dve", t9, AI, AI, ALU.mult, R_, W_)
        kb.tt("dve", den, den, t9, ALU.add, R_, W_)
        kb.op("dve", lambda g: g.reciprocal(den, den), R_, W_)
        kb.tt("dve", zre, lre, AR, ALU.mult, R_, W_)
        kb.tt("dve", t9, lim, AI, ALU.mult, R_, W_)
        kb.tt("dve", zre, zre, t9, ALU.add, R_, W_)
        kb.tt("dve", zre, zre, den, ALU.mult, R_, W_)
        kb.tt("dve", zim, lim, AR, ALU.mult, R_, W_)
        kb.tt("dve", t9, lre, AI, ALU.mult, R_, W_)
        kb.tt("dve", zim, zim, t9, ALU.subtract, R_, W_)
        kb.tt("dve", zim, zim, den, ALU.mult, R_, W_)
        kb.tt("dve", bre, zre, BRT, ALU.mult, R_, W_)
        kb.tt("dve", t9, zim, BIT, ALU.mult, R_, W_)
        kb.tt("dve", bre, bre, t9, ALU.subtract, R_, W_)
        kb.tt("dve", bim, zre, BIT, ALU.mult, R_, W_)
        kb.tt("dve", t9, zim, BRT, ALU.mult, R_, W_)
        kb.tt("dve", bim, bim, t9, ALU.add, R_, W_)
        kb.cp("dve", Bm[:, :, 0, 0:64], bre, R_, [B_Bm]); kb.cp("dve", Bm[:, :, 0, 64:128], bim, R_, [B_Bm])
        kb.cp("dve", Bm[:, :, 1, 0:64], bim, R_, [B_Bm])
        kb.ts("dve", Bm[:, :, 1, 64:128], bre, -1.0, ALU.mult, R_, [B_Bm])
        kb.cp("dve", BmX[64:128, :, :, :], Bm[64:128, :, :, :], [B_Bm], [B_Bm])
        kb.memset("dve", BmX[64:96, :, :, :], 0.0, [B_Bm])
        AR2, AI2, LDT2 = [p2[:, i, :] for i in range(3)]
        dt2 = tm[:, 15, 0:16]; th = tm[:, 15, 16:32]; thi = tm[:, 15, 32:48]
        kb.act(dt2, LDT2, AF.Exp, R_, W_)
        kb.tt("dve", rr[:], dt2, AR2, ALU.mult, R_, [B_rr])
        kb.act(rr[:], rr[:], AF.Exp, [B_rr], [B_rr])
        kb.tt("dve", th, dt2, AI2, ALU.mult, R_, W_)
        for g in range(16):
            kb.ts("dve", tm[:, 16, :], cs("iota")[:, 0:256], th[:, g:g + 1], ALU.mult, R_ + [B_cst], W_)
            kb.ts("dve", tm[:, 17, 0:1], cs("iota")[:, 256:257], th[:, g:g + 1], ALU.mult, R_ + [B_cst], W_)
            sincos(tab[:, g, 1, 0:256], tab[:, g, 0, 0:256], tm[:, 16, :], 256)
            sincos(tm[:, 18, 0:1], tm[:, 18, 1:2], tm[:, 17, 0:1], 1)
            kb.cp("dve", tab[:, g, 1, 256:257], tm[:, 18, 0:1], R_, [B_tab])
            kb.cp("dve", tab[:, g, 0, 256:257], tm[:, 18, 1:2], R_, [B_tab])
            kb.tt("dve", tm[:, 18, 2:3], tm[:, 18, 0:1], cs("sgn"), ALU.mult, R_ + [B_cst], W_)
            kb.cp("dve", cs2[:, g, 0:1], tm[:, 18, 1:2], R_, [B_Rot])
            kb.cp("dve", cs2[:, g, 1:2], tm[:, 18, 2:3], R_, [B_Rot])
        kb.ts("dve", Cm[:, :, 0, :], c5[:, 0, :, :], cs("sgn"), ALU.mult, [B_p, B_cst], [B_Cm])
        kb.ts("dve", Cm[:, :, 1, :], c5[:, 1, :, :], -1.0, ALU.mult, [B_p], [B_Cm])
    if stop_after == "prep":
        dbg = {}
        for nm, t_, shp, dt_o in (("tab", tab, [128, 16 * 2 * (T5 + 1)], BF16), ("Bm", Bm, [128, 4 * 2 * 128], BF16),
                                 ("Cm", Cm, [128, 16 * 2 * 64], BF16), ("Rot", cs2, [128, 32], F32),
                                 ("rr", rr, [128, 16], F32), ("skA", skA, [128, 2 * WA], BF16), ("skD", skD, [128, 2 * WD], BF16),
                                 ("lbv", lbv, [128, 2], F32)):
            o = nc.dram_tensor("dbg_" + nm, shp, dt_o, kind="ExternalOutput").ap()
            allb = [B_tab, B_Bm, B_Cm, B_Rot, B_rr, B_sk, B_lb]
            flat = t_[:] if len(t_.shape) == 2 else t_[:].rearrange({3: "p a b -> p (a b)", 4: "p a b c -> p (a b c)"}[len(t_.shape)])
            kb.dma("sp", o[:, :], flat, allb, [], 7)
        kb.finish("sp", [])
        kb.eng["sp"].wait_ge(kb.sem[("d", 7)], kb.cnt[("d", 7)])
        return nc, kb, None

    kb.barrier()
    xst = sb("xst", [128, 8, 128]); B_xst = Buf(); v12 = sb("v12", [128, 2]); B_v12 = Buf()
    xbf = sb("xbf", [128, 8, 512], BF16); B_x = Buf()
    QA = [sb("QA%d" % h, [96, 512], BF16) for h in range(2)]; B_QA = [Buf(), Buf()]
    qf = [sb("qf%d" % h, [64, 512]) for h in range(2)]; B_qf = [Buf(), Buf()]
    GGA = [sb("GGA%d" % h, [64, 512], BF16) for h in range(2)]; B_GGA = [Buf(), Buf()]
    GGD = [sb("GGD%d" % h, [64, 512], BF16) for h in range(2)]; B_GGD = [Buf(), Buf()]
    QD = sb("QD", [128, 512], BF16); B_QD = Buf()
    sq = sb("sq", [128, 512], BF16); sg = sb("sg", [128, 512], BF16); gC = sb("gC", [128, 512], BF16); B_sq = Buf(); B_sg = Buf(); B_gC = Buf()
    ubf = sb("ubf", [128, 4, 512], BF16); uf = ubf; gB = sb("gB", [128, 2, 512], BF16)
    B_ubf = Buf(); B_uf = Buf(); B_gB = Buf()
    Vc = sb("Vc", [128, 4, 128], BF16); B_Vc = Buf()
    Sb = [sb("Sb%d" % i, [128, 512]) for i in range(2)]; B_Sb = [Buf(), Buf()]
    Pb = [sb("Pb%d" % i, [128, 512], BF16) for i in range(2)]; B_Pb = [Buf(), Buf()]
    msel = sb("msel", [128, 32], BF16); top8 = sb("top8", [128, 8]); B_msel = Buf(); B_top8 = Buf()
    osq = Sb[1]; scl = sb("scl", [128, 512]); ftmp = Sb[0]
    B_osq = B_Sb[1]; B_scl = Buf(); B_ftmp = B_Sb[0]
    ygo_ = [sb("ygo%d" % i, [128, 512], BF16) for i in range(1)]; B_ygo_ = [Buf() for _ in range(1)]
    ygo = [ygo_[0] for i in range(7)]; B_ygo = [B_ygo_[0] for i in range(7)]
    t1 = sb("t1", [128, T5]); t2 = sb("t2", [128, T5]); rot = sb("rot", [128, T5]); Gs = sb("Gs", [128, T5])
    cG = sb("cG", [128, T5], BF16); sG = sb("sG", [128, T5], BF16)
    B_t1 = Buf(); B_t2 = Buf(); B_rot = Buf(); B_G = Buf(); B_cG = Buf(); B_sG = Buf()
    yv1 = sb("yv", [128, T5]); zz = sb("zz", [128, 2, T5]); zb = sb("zb", [128, 4, T5], BF16); gt = sb("gt", [128, T5])
    B_yv = Buf(); B_zz = Buf(); B_zb = Buf(); B_gt = Buf()
    hf_ = t1; hg = t2; hk = sb("hk", [128, T5]); hb = rot; hd = Gs
    hE = sb("hE", [128, T5])
    hqt = sb("hqt", [128, T5], BF16); hkt = sb("hkt", [128, 2, T5], BF16); hqp = sb("hqp", [128, T5], BF16); hkh = sb("hkh", [128, T5], BF16)
    khT = sb("khT", [128, 2, 128], BF16); pm = sb("pm", [128, 128], BF16); ebl = sb("ebl", [128, 4])
    B_hf = B_t1; B_hg = B_t2; B_hk = Buf(); B_hb = B_rot; B_hd = B_G; B_hE = Buf()
    B_hqt = Buf(); B_hkt = Buf(); B_hqp = Buf(); B_hkh = Buf(); B_khT = Buf(); B_pm = Buf(); B_ebl = Buf()

    kb.memset("pool", VA[:, :, :, 64:65], 1.0, B_VA)
    kb.memset("pool", VD[:, :, :, 64:65], 1.0, B_VD)
    for h in range(2):
        kb.memset("pool", ksum[h][:], 0.0, [B_ksum[h]])
        kb.memset("pool", gsb[h][:], -1e30, [B_gsb[h]])
    kb.memset("pool", hst[:], 0.0, [B_hst]); kb.memset("pool", hstb[:], 0.0, [B_hstb]); kb.memset("pool", hkt[:], 0.0, [B_hkt])
    xv = xT.rearrange("(k p) t -> p k t", p=128)

    def load_x(j):
        for half in range(4):
            c0 = 512 * j + 128 * half
            kb.dma("sp", xst[:], xv[:, :, c0:c0 + 128], [], [B_xst], 0)
            kb.cp("pool", xbf[:, :, 128 * half:128 * half + 128], xst[:], [B_xst], [B_x])

    def fm_group(col0, M):
        p_, B_ = pingpong()
        for dk in range(8):
            kb.mm(p_[0:M, :], wbf[:, dk, col0:col0 + M], xbf[:, dk, :], [B_w, B_x], [B_], start=(dk == 0), stop=(dk == 7))
        return p_[0:M, :], B_

    def finalize(o_ps, B_o, nrow, wmat, GG, B_GG, gcol, yi, rows, t0, ncol, pad16=False):
        nr_in = o_ps.shape[0]
        kb.act(osq[0:nr_in, 0:ncol], o_ps, AF.Square, [B_o], [B_osq])
        kb.mm(ps[3][0:nrow, 0:ncol], wmat, osq[0:nr_in, 0:ncol], [B_osq, B_cst], [B_ps[3]])
        return _fin2(o_ps[0:nrow, :], B_o, nrow, GG, B_GG, gcol, yi, rows, t0, ncol, pad16, 1.0)

    def _fin2(o_in, B_o, nrow, GG, B_GG, gcol, yi, rows, t0, ncol, pad16, sc_):
        if sc_ == 1.0:
            kb.act(scl[0:nrow, 0:ncol], ps[3][0:nrow, 0:ncol], AF.Sqrt, [B_ps[3]], [B_scl])
        else:
            kb.act(scl[0:nrow, 0:ncol], ps[3][0:nrow, 0:ncol], AF.Sqrt, [B_ps[3], B_cst], [B_scl], scale=sc_, bias=eps[0:nrow, :])
        kb.op("dve", lambda g: g.reciprocal(scl[0:nrow, 0:ncol], scl[0:nrow, 0:ncol]), [B_scl], [B_scl])
        kb.tt("dve", ftmp[0:nrow, 0:ncol], o_in, scl[0:nrow, 0:ncol], ALU.mult, [B_o, B_scl], [B_ftmp])
        kb.stt(ygo[yi][0:nrow, 0:ncol], ftmp[0:nrow, 0:ncol], gcol, GG, ALU.mult, ALU.mult, [B_ftmp, B_GG, B_small], [B_ygo[yi]])
        if pad16:
            for a in range(4):
                kb.dma("sp", yg[rows + 16 * a:rows + 16 * a + 16, t0:t0 + ncol], ygo[yi][32 * a:32 * a + 16, 0:ncol], [B_ygo[yi]], [], 3)
        else:
            kb.dma("sp", yg[rows:rows + nrow, t0:t0 + ncol], ygo[yi][0:nrow, 0:ncol], [B_ygo[yi]], [], 3)

    load_x(0)
    for j in range(NST):
        t0 = 512 * j
        for h in range(2):
            p_, B_ = fm_group(C_QA + 64 * h, 64)
            kb.act(QA[h][0:64, :], p_, AF.Copy, [B_], [B_QA[h]], scale=0.125)
            kb.cp("dve", qf[h][:], p_, [B_], [B_qf[h]])
            p_, B_ = fm_group(C_KA + 64 * h, 64)
            kb.act(KA[h][0:64, t0:t0 + 512], p_, AF.Copy, [B_], [B_KA[h][j]])
            kb.op("dve", lambda g: g.tensor_reduce(ksum[h][:, 2 * j:2 * j + 2], p_.rearrange("p (b s) -> p b s", b=2), AX.X, ALU.add),
                  [B_], [B_ksum[h]])
            p_, B_ = fm_group(C_GA + 64 * h, 64)
            kb.act(GGA[h][:], p_, AF.Silu, [B_], [B_GGA[h]])
            p_, B_ = fm_group(C_GD + 64 * h, 64)
            kb.act(GGD[h][:], p_, AF.Silu, [B_], [B_GGD[h]])
        p_, B_ = fm_group(C_QD, 128)
        kb.act(QD[:], p_, AF.Copy, [B_], [B_QD], scale=0.125)
        p_, B_ = fm_group(C_KD, 128)
        kb.act(KD[:, j % 5, :], p_, AF.Copy, [B_], [B_KD[j % 5]])
        p_, B_ = fm_group(C_QC, 128)
        kb.act(sq[:], p_, AF.Silu, [B_], [B_sq])
        p_, B_ = fm_group(C_FC, 128)
        kb.act(sg[:], p_, AF.Sigmoid, [B_], [B_sg])
        p_, B_ = fm_group(C_GC, 128)
        kb.act(gC[:], p_, AF.Silu, [B_], [B_gC])
        for hi in range(4):
            p_, B_ = fm_group(C_U + 128 * hi, 128)
            kb.act(ubf[:, hi, :], p_, AF.Copy, [B_], [B_ubf])
        for ho in range(2):
            p_, B_ = fm_group(C_GB + 128 * ho, 128)
            kb.act(gB[:, ho, :], p_, AF.Silu, [B_], [B_gB])
        for tt_ in range(4):
            p_, B_ = pingpong()
            for dk in range(8):
                kb.mm(p_[:, 0:384], xbf[:, dk, 128 * tt_:128 * tt_ + 128], wbf[:, dk, C_TM:C_TM + 384], [B_w, B_x], [B_],
                      start=(dk == 0), stop=(dk == 7))
            kt = 4 * j + tt_
            kb.cp("dve", VA[:, kt, :, 0:64], p_[:, 0:128].rearrange("p (h d) -> p h d", h=2), [B_], [B_VA[j]])
            kb.cp("dve", VD[:, kt % 20, :, 0:64], p_[:, 128:256].rearrange("p (h d) -> p h d", h=2), [B_], [B_VD[j % 5]])
            kb.act(Vc[:, tt_, :], p_[:, 256:384], AF.Copy, [B_], [B_Vc])
        if j + 1 < NST:
            load_x(j + 1)
        if "A" in branches:
          for h in range(2):
            for i in range(4):
                own = 2 * j + i // 2
                if own == 0:
                    kb.memset("dve", msel[:], NEGM, [B_msel])
                else:
                    kb.mm(ps[3][:, 0:32], qf[h][:, 128 * i:128 * i + 128], ksum[h][:, 0:32], [B_qf[h], B_ksum[h]], [B_ps[3]])
                    kb.cp("dve", gsb[h][:, 0:own], ps[3][:, 0:own], [B_ps[3]], [B_gsb[h]])
                    kb.op("dve", lambda g: g.max(top8[:], gsb[h][:]), [B_gsb[h]], [B_top8])
                    kk_ = min(2, own - 1)
                    kb.ts("dve", msel[:], gsb[h][:], top8[:, kk_:kk_ + 1], ALU.is_lt, [B_gsb[h], B_top8], [B_msel], s2=NEGM, op1=ALU.mult)
                kb.memset("dve", msel[:, own:own + 1], 0.0, [B_msel])
                kb.tr(psb[64:96, 0:128], msel[:], idb[:], [B_msel, B_idb], [B_psb])
                kb.act(QA[h][64:96, 128 * i:128 * i + 128], psb[64:96, 0:128], AF.Copy, [B_psb], [B_QA[h]])
            nkt = 4 * (j + 1)
            for kt in range(nkt):
                dlt = t0 - 128 * kt
                p_, B_ = pingpong()
                kb.mm(p_[:, :], KA[h][0:96, 128 * kt:128 * kt + 128], QA[h][0:96, :], [B_KA[h][kt // 4], B_E, B_QA[h]], [B_])
                bi = kt % 2
                if dlt <= 1536:
                    kb.tt("dve", Sb[bi][:], p_[:, :], skA[:, h, dlt + SKOFF:dlt + SKOFF + 512], ALU.add, [B_, B_sk], [B_Sb[bi]])
                    kb.act(Pb[bi][:], Sb[bi][:], AF.Exp, [B_Sb[bi]], [B_Pb[bi]])
                else:
                    kb.act(Pb[bi][:], p_[:, :], AF.Exp, [B_, B_small], [B_Pb[bi]], bias=b31[:, h:h + 1])
                kb.mm(ps[2][0:65, :], VA[:, kt, h, :], Pb[bi][:], [B_VA[kt // 4], B_Pb[bi]], [B_ps[2]], start=(kt == 0), stop=(kt == nkt - 1))
            finalize(ps[2][0:65, :], B_ps[2], 64, cs("wb65")[0:65, :], GGA[h][:], B_GGA[h], gain[0:64, h:h + 1], h, 64 * h, t0, 512)
        if "D" in branches:
          for h in range(2):
            kts = [kt for kt in range(4 * j - 16, 4 * j + 4) if kt >= 0]
            for n_, kt in enumerate(kts):
                dlt = t0 - 128 * kt
                p_, B_ = pingpong()
                js = kt // 4
                kb.mm(p_[:, :], KD[64 * h:64 * h + 64, js % 5, 128 * (kt % 4):128 * (kt % 4) + 128], QD[64 * h:64 * h + 64, :],
                      [B_KD[js % 5], B_QD], [B_])
                bi = kt % 2
                kb.tt("dve", Sb[bi][:], p_[:, :], skD[:, h, dlt + SKOFF:dlt + SKOFF + 512], ALU.add, [B_, B_sk], [B_Sb[bi]])
                kb.act(Pb[bi][:], Sb[bi][:], AF.Exp, [B_Sb[bi]], [B_Pb[bi]])
                kb.mm(ps[2][0:65, :], VD[:, kt % 20, h, :], Pb[bi][:], [B_VD[js % 5], B_Pb[bi]], [B_ps[2]], start=(n_ == 0), stop=(n_ == len(kts) - 1))
            finalize(ps[2][0:65, :], B_ps[2], 64, cs("wb65")[0:65, :], GGD[h][:], B_GGD[h], gain[0:64, 5 + h:6 + h], 5 + h, 384 + 64 * h, t0, 512)
        if "B" in branches:
          for st in range(2):
            c0 = T5 * st
            first = (j == 0 and st == 0)
            for g in range(16):
                a, hi = g % 4, g // 4
                if a < 3:
                    bl_, r0, r1 = Bm, 32 * a, 32 * a + 32
                else:
                    bl_, r0, r1 = BmX, 64, 128
                kb.mm(ps[4][:, 0:T5], bl_[r0:r1, hi, 0, :], ubf[r0:r1, hi, c0:c0 + T5], [B_Bm, B_ubf], [B_ps[4]])
                kb.mm(ps[4][:, T5:2 * T5], bl_[r0:r1, hi, 1, :], ubf[r0:r1, hi, c0:c0 + T5], [B_Bm, B_ubf], [B_ps[4]])
                kb.tt("dve", t1[:], ps[4][:, 0:T5], tab[:, g, 0, 0:T5], ALU.mult, [B_ps[4], B_tab], [B_t1])
                kb.tt("dve", t2[:], ps[4][:, T5:2 * T5], tab[:, g, 1, 0:T5], ALU.mult, [B_ps[4], B_tab], [B_t2])
                kb.tt("pool", rot[:], t1[:], t2[:], ALU.add, [B_t1, B_t2], [B_rot])
                init = 0.0 if first else carry[:, g:g + 1]
                kb.op("dve", lambda g_: g_.tensor_tensor_scan(Gs[:], rr[:, g:g + 1].to_broadcast([128, T5]), rot[:], init, ALU.mult, ALU.add),
                      [B_rr, B_rot] + ([] if first else [B_carry[g]]), [B_G])
                kb.tt("dve", v12[:], Gs[:, T5 - 1:T5].to_broadcast([128, 2]), cs2[:, g, :], ALU.mult, [B_Rot, B_G], [B_v12])
                kb.mm(ps[3][:, 32:33], cs("id"), v12[:, 0:1], [B_cst, B_v12], [B_ps[3]], start=True, stop=False)
                kb.mm(ps[3][:, 32:33], cs("sw"), v12[:, 1:2], [B_cst, B_v12], [B_ps[3]], start=False, stop=True)
                kb.act(carry[:, g:g + 1], ps[3][:, 32:33], AF.Copy, [B_ps[3]], [B_carry[g]])
                kb.tt("pool", cG[:], Gs[:], tab[:, g, 0, 0:T5], ALU.mult, [B_G, B_tab], [B_cG])
                kb.tt("pool", sG[:], Gs[:], tab[:, g, 1, 0:T5], ALU.mult, [B_G, B_tab], [B_sG])
                ycol = slice((hi % 2) * T5, (hi % 2) * T5 + T5)
                if a < 2:
                    yo = ps[5][32 * a:32 * a + 32, ycol]
                    kb.mm(yo, Cm[:, g, 0, 0:32], cG[:], [B_Cm, B_cG], [B_ps[5]], start=True, stop=False)
                    kb.mm(yo, Cm[:, g, 1, 0:32], sG[:], [B_Cm, B_sG], [B_ps[5]], start=False, stop=True)
                else:
                    yo = ps[5][64:128, ycol]
                    kb.mm(yo, Cm[:, g, 0, :], cG[:], [B_Cm, B_cG], [B_ps[5]], start=(a == 2), stop=False)
                    kb.mm(yo, Cm[:, g, 1, :], sG[:], [B_Cm, B_sG], [B_ps[5]], start=False, stop=(a == 3))
                if a == 3 and hi % 2 == 1:
                    for hh in (hi - 1, hi):
                        Y = ps[5][:, (hh % 2) * T5:(hh % 2) * T5 + T5]
                        kb.stt(yv1[:], uf[:, hh, c0:c0 + T5], dsk[:, hh:hh + 1], Y, ALU.mult, ALU.add, [B_ubf, B_small, B_ps[5]], [B_yv])
                        kb.act(gt[:], yv1[:], AF.Square, [B_yv], [B_gt])
                        kb.ts("dve", gt[:], gt[:], 0.044715, ALU.mult, [B_gt], [B_gt], s2=1.0, op1=ALU.add)
                        kb.tt("dve", gt[:], gt[:], yv1[:], ALU.mult, [B_gt, B_yv], [B_gt])
                        kb.act(gt[:], gt[:], AF.Sigmoid, [B_gt], [B_gt], scale=1.5957691216057308)
                        if hh < 2:
                            kb.tt("dve", zz[:, hh, :], yv1[:], gt[:], ALU.mult, [B_gt, B_yv], [B_zz])
                            kb.cp("pool", zb[:, hh, :], zz[:, hh, :], [B_zz], [B_zb])
                        else:
                            kb.tt("dve", zb[:, hh, :], yv1[:], gt[:], ALU.mult, [B_gt, B_yv], [B_zb])
            for ho in range(2):
                go = ps[4][:, ho * T5:ho * T5 + T5]
                for hi in range(4):
                    kb.mm(go, gw[:, hi, 128 * ho:128 * ho + 128], zb[:, hi, :], [B_gw, B_zb], [B_ps[4]], start=(hi == 0), stop=(hi == 3))
                kb.act(gt[:], go, AF.Sigmoid, [B_ps[4], B_small], [B_gt], bias=glub[:, ho:ho + 1])
                kb.tt("dve", t1[:], zz[:, ho, :], gt[:], ALU.mult, [B_zz, B_gt], [B_t1])
                kb.act(osq[:, 0:T5], t1[:], AF.Square, [B_t1], [B_osq])
                kb.mm(ps[3][:, 0:T5], cs("ones"), osq[:, 0:T5], [B_osq, B_cst], [B_ps[3]])
                _fin2(t1[:], B_t1, 128, gB[:, ho, c0:c0 + T5], B_gB, gain[:, 2 + ho:3 + ho], 2 + ho, 128 + 64 * ho, t0 + c0, T5, True, 1.0 / 64)
        if "C" in branches:
          for st in range(2):
            c0 = T5 * st
            kb.ts("dve", hf_[:], sg[:, c0:c0 + T5], lbv[:, 1:2], ALU.mult, [B_sg, B_lb], [B_hf], s2=lbv[:, 0:1], op1=ALU.add)
            kb.act(hg[:], hf_[:], AF.Ln, [B_hf], [B_hg])
            kb.ts("dve", hk[:], hf_[:], -1.0, ALU.mult, [B_hf], [B_hk], s2=1.0, op1=ALU.add)
            kb.op("dve", lambda g_: g_.tensor_tensor_scan(hb[:], cs("seg"), hg[:], 0.0, ALU.mult, ALU.add), [B_cst, B_hg], [B_hb])
            b3 = hb[:].rearrange("p (c s) -> p c s", s=64)
            d3 = hd[:].rearrange("p (c s) -> p c s", s=64)
            kb.tt("dve", d3, b3, b3[:, :, 31:32].to_broadcast([128, 4, 64]), ALU.subtract, [B_hb], [B_hd])
            kb.act(hE[:], hd[:], AF.Exp, [B_hd], [B_hE])
            kb.stt(hqt[:], sq[:, c0:c0 + T5], 0.125, hE[:], ALU.mult, ALU.mult, [B_sq, B_hE], [B_hqt])
            kb.act(hE[:], hd[:], AF.Exp, [B_hd, B_hqt], [B_hE], scale=-1.0)
            for h in range(2):
                kb.tt("dve", hkt[64 * h:64 * h + 64, h, :], hk[64 * h:64 * h + 64, :], hE[64 * h:64 * h + 64, :], ALU.mult, [B_hk, B_hE], [B_hkt])
            kb.act(hE[:], hb[:], AF.Exp, [B_hb, B_hkt], [B_hE])
            kb.stt(hqp[:], sq[:, c0:c0 + T5], 0.125, hE[:], ALU.mult, ALU.mult, [B_sq, B_hE], [B_hqp])
            kb.tt("dve", d3, b3, b3[:, :, 63:64].to_broadcast([128, 4, 64]), ALU.subtract, [B_hb, B_hqp], [B_hd])
            kb.act(hE[:], hd[:], AF.Exp, [B_hd, B_hqp], [B_hE], scale=-1.0)
            kb.tt("dve", hkh[:], hk[:], hE[:], ALU.mult, [B_hk, B_hE], [B_hkh])
            kb.act(ebl[:], b3[:, :, 63], AF.Exp, [B_hb], [B_ebl])
            for t2_ in range(2):
                kb.tr(psb[:, 0:128], hkh[:, 128 * t2_:128 * t2_ + 128], idb[:], [B_hkh, B_idb], [B_psb])
                kb.cp("dve", khT[:, t2_, :], psb[:, 0:128], [B_psb], [B_khT])
            for c in range(4):
                t2_, pb = c // 2, 64 * (c % 2)
                vt = 2 * st + t2_
                for h in range(2):
                    kb.mm(ps[6][pb:pb + 64, 64 * h:64 * h + 64], hkt[:, h, 64 * c:64 * c + 64], hqt[:, 64 * c:64 * c + 64],
                          [B_hkt, B_hqt], [B_ps6[0]])
                kb.tt("dve", pm[pb:pb + 64, :], ps[6][pb:pb + 64, 0:128], cs("m01")[pb:pb + 64, :], ALU.mult, [B_ps6[0], B_cst], [B_pm])
                for h in range(2):
                    oo = ps[6][64 * h:64 * h + 64, 128 + 64 * c:128 + 64 * c + 64]
                    kb.mm(oo, Vc[pb:pb + 64, vt, 64 * h:64 * h + 64], pm[pb:pb + 64, 64 * h:64 * h + 64], [B_Vc, B_pm], [B_ps6[1]], start=True, stop=False)
                    kb.mm(oo, hstb[:, h, :], hqp[:, 64 * c:64 * c + 64], [B_hstb, B_hqp], [B_ps6[1]], start=False, stop=True)
                for h in range(2):
                    kb.mm(ps[6][64 * h:64 * h + 64, 384:448], khT[pb:pb + 64, t2_, 64 * h:64 * h + 64], Vc[pb:pb + 64, vt, 64 * h:64 * h + 64],
                          [B_khT, B_Vc], [B_ps6[2]])
                kb.stt(hst[:], hst[:], ebl[:, c:c + 1], ps[6][:, 384:448], ALU.mult, ALU.add, [B_hst, B_ebl, B_ps6[2]], [B_hst])
                for h in range(2):
                    kb.act(hstb[64 * h:64 * h + 64, h, :], hst[64 * h:64 * h + 64, :], AF.Copy, [B_hst], [B_hstb])
            finalize(ps[6][:, 128:384], B_ps6[1], 128, cs("wb2"), gC[:, c0:c0 + T5], B_gC, gain[:, 4:5], 4, 256, t0 + c0, T5)
    kb.wait_all_dma("sp")
    return nc, kb, None


def build_stageB(NT):
    nc = bass.Bass("TRN2", target_bir_lowering=False)
    kb = KB(nc)
    dr = lambda n, sh, dt=F32, kind="ExternalInput": nc.dram_tensor(n, sh, dt, kind=kind).ap()
    ygT = dr("ygT", [1024, NT], BF16)
    xT = dr("xT", [1024, NT])
    wo = dr("wo", [1024, 1024])
    lnp = dr("lnp", [128, 16])
    ones_d = dr("ones", [128, 128])
    out = dr("x1T", [1024, NT], kind="ExternalOutput")
    sb = lambda n, sh, dt=F32: nc.alloc_sbuf_tensor("s_" + n, sh, dt)
    wob = sb("wob", [128, 8, 1024], BF16); B_w = Buf()
    stg = sb("stg", [128, 1024]); B_stg = Buf()
    lnv = sb("lnv", [128, 16]); B_ln = Buf()
    ones = sb("ones", [128, 128]); B_ones = Buf()
    epsb = sb("epsb", [128, 1]); B_eps = Buf()
    ygt = sb("ygt", [128, 8, 512], BF16); B_yg = Buf()
    xt = sb("xt", [128, 8, 512]); B_xt = Buf()
    hT = sb("hT", [128, 8, 512]); B_h = Buf()
    hsq = sb("hsq", [128, 512]); B_hsq = Buf()
    mean = sb("mean", [128, 512]); var = sb("var", [128, 512]); B_mean = Buf(); B_var = Buf()
    tmp = [sb("tmp%d" % i, [128, 512]) for i in range(2)]; B_tmp = [Buf(), Buf()]
    ob = [sb("ob%d" % i, [128, 512]) for i in range(2)]; B_ob = [Buf(), Buf()]
    ps = [nc.alloc_psum_tensor("ps%d" % i, [128, 512], F32) for i in range(4)]
    B_ps = [Buf(True) for _ in range(4)]
    wv = wo.rearrange("(k p) n -> p k n", p=128)
    for kc in range(8):
        kb.dma("sp", stg[:], wv[:, kc, :], [], [B_stg], 1)
        kb.cp("pool" if kc % 2 else "dve", wob[:, kc, :], stg[:], [B_stg], [B_w])
    kb.dma("sp", lnv[:], lnp[:, :], [], [B_ln], 2)
    kb.dma("sp", ones[:], ones_d[:, :], [], [B_ones], 2)
    kb.memset("dve", epsb[:], 1e-5, [B_eps])
    yv = ygT.rearrange("(k p) t -> p k t", p=128)
    xv = xT.rearrange("(k p) t -> p k t", p=128)
    ov = out.rearrange("(k p) t -> p k t", p=128)
    for it in range(NT // 512):
        c0 = 512 * it
        kb.dma("sp", ygt[:], yv[:, :, c0:c0 + 512], [], [B_yg], 0)
        kb.dma("sp", xt[:], xv[:, :, c0:c0 + 512], [], [B_xt], 0)
        for co in range(8):
            p_, B_ = ps[co % 2], B_ps[co % 2]
            for kc in range(8):
                kb.mm(p_[:, :], wob[:, kc, 128 * co:128 * co + 128], ygt[:, kc, :], [B_w, B_yg], [B_], start=(kc == 0), stop=(kc == 7))
            kb.stt(hT[:, co, :], xt[:, co, :], ALPHA, p_[:, :], ALU.mult, ALU.add, [B_xt, B_], [B_h])
            kb.act(hsq[:], hT[:, co, :], AF.Square, [B_h], [B_hsq])
            kb.mm(ps[2][:, :], ones[:], hT[:, co, :], [B_ones, B_h], [B_ps[2]], start=(co == 0), stop=(co == 7))
            kb.mm(ps[3][:, :], ones[:], hsq[:], [B_ones, B_hsq], [B_ps[3]], start=(co == 0), stop=(co == 7))
        kb.ts("dve", mean[:], ps[2][:, :], 1.0 / 1024, ALU.mult, [B_ps[2]], [B_mean])
        kb.tt("dve", var[:], mean[:], mean[:], ALU.mult, [B_mean], [B_var])
        kb.stt(var[:], ps[3][:, :], 1.0 / 1024, var[:], ALU.mult, ALU.subtract, [B_ps[3], B_var], [B_var])
        kb.act(var[:], var[:], AF.Sqrt, [B_var, B_eps], [B_var], bias=epsb[:])
        kb.op("dve", lambda g: g.reciprocal(var[:], var[:]), [B_var], [B_var])
        for co in range(8):
            i = co % 2
            kb.tt("dve", tmp[i][:], hT[:, co, :], mean[:], ALU.subtract, [B_h, B_mean], [B_tmp[i]])
            kb.tt("pool", tmp[i][:], tmp[i][:], var[:], ALU.mult, [B_tmp[i], B_var], [B_tmp[i]])
            kb.act(ob[i][:], tmp[i][:], AF.Identity, [B_tmp[i], B_ln], [B_ob[i]], scale=lnv[:, co:co + 1], bias=lnv[:, 8 + co:9 + co])
            kb.dma("sp", ov[:, co, c0:c0 + 512], ob[i][:], [B_ob[i]], [], 3)
    kb.wait_all_dma("sp")
    return nc, kb


_PROG = {}


def _get_prog(name, *args):
    key = (name,) + args
    if key not in _PROG:
        if name == "A":
            _PROG[key] = build_stageA(*args)[0]
        else:
            _PROG[key] = build_stageB(*args)[0]
    return _PROG[key]


def _wout_rows():
    rows = []
    for hf in range(2):
        for br in range(4):
            rows.extend(range(256 * br + 128 * hf, 256 * br + 128 * hf + 128))
    return np.array(rows)


def kernel(x, w_in, rel_bias, s5_a_re, s5_a_im, s5_log_dt, s5_b_re, s5_b_im, s5_c_re, s5_c_im,
           s5_d, s5_glu_w, s5_glu_b, hgrn_lower, branch_gain, w_out, ln_g, ln_b):
    f = lambda a: np.asarray(a, dtype=np.float32)
    x, w_in, rel_bias = f(x), f(w_in), f(rel_bias)
    P = dict(s5_a_re=f(s5_a_re), s5_a_im=f(s5_a_im), s5_log_dt=f(s5_log_dt), s5_b_re=f(s5_b_re), s5_b_im=f(s5_b_im),
             s5_c_re=f(s5_c_re), s5_c_im=f(s5_c_im), s5_d=f(s5_d), s5_glu_w=f(s5_glu_w), s5_glu_b=f(s5_glu_b),
             hgrn_lower=f(hgrn_lower), branch_gain=f(branch_gain))
    w_out, ln_g, ln_b = f(w_out), f(ln_g), f(ln_b)
    Bt, S, D = x.shape
    NT = S // 2
    xT = [np.ascontiguousarray(x[b].T) for b in range(Bt)]
    rows = _wout_rows()
    ones = np.ones((128, 128), np.float32)
    for l in range(DEPTH):
        ncA = _get_prog("A", S, 0)
        mapsA = [host_stageA_inputs(l, c // 2, c % 2, S, xT[c // 2], w_in, rel_bias, **P) for c in range(8)]
        resA = run_bass_kernel_spmd(ncA, mapsA, core_ids=list(range(8))).results
        yg = [np.asarray(r["yg"]) for r in resA]
        ncB = _get_prog("B", NT)
        wo = np.ascontiguousarray(w_out[l][rows, :])
        lnp = np.concatenate([ln_g[l].reshape(8, 128).T, ln_b[l].reshape(8, 128).T], axis=1).astype(np.float32)
        mapsB = []
        for c in range(8):
            b, th = c // 2, c % 2
            sl = slice(th * NT, (th + 1) * NT)
            ygT = np.ascontiguousarray(np.concatenate([yg[2 * b][:, sl], yg[2 * b + 1][:, sl]], axis=0))
            mapsB.append({"ygT": ygT, "xT": np.ascontiguousarray(xT[b][:, sl]), "wo": wo, "lnp": np.ascontiguousarray(lnp), "ones": ones})
        resB = run_bass_kernel_spmd(ncB, mapsB, core_ids=list(range(8))).results
        xT = [np.ascontiguousarray(np.concatenate([np.asarray(resB[2 * b]["x1T"]), np.asarray(resB[2 * b + 1]["x1T"])], axis=1))
              for b in range(Bt)]
    return np.ascontiguousarray(np.stack([xT[b].T for b in range(Bt)], axis=0)).astype(np.float32)
```
